# Optimizing a Trainium2 kernel written in Bass

```python
import math
import jax, jax.numpy as jnp
from jax import lax
import numpy as np

D_MODEL = 2048
BATCH = 2
SEQ = 16384
DEPTH = 1
DEC_BATCH = 8
DEC_SEQ = 2048
PAST_LEN = 128

MLA_HEADS = 8
Q_LORA = 512
KV_LORA = 512
QK_NOPE = 128
QK_ROPE = 64
V_HEAD = 128
ROPE_THETA = 10000.0
Q_BLOCK = 128
GDN_HEADS = 8
GDN_DK = 128
GDN_DV = 128
GDN_QKV = GDN_HEADS * (2 * GDN_DK + GDN_DV)
CONV_K = 5
CHUNK = 64
D_ATTN = MLA_HEADS * V_HEAD
D_GDN = GDN_HEADS * GDN_DV
D_MIX = D_ATTN + D_GDN
IN_SPLITS = (Q_LORA, KV_LORA, QK_ROPE, GDN_QKV, D_GDN, 2 * GDN_HEADS, 2 * GDN_HEADS)
N_IN = Q_LORA + KV_LORA + QK_ROPE + GDN_QKV + D_GDN + 4 * GDN_HEADS
N_GROUPS = 8
EXPERTS_PER_GROUP = 8
N_EXPERTS = N_GROUPS * EXPERTS_PER_GROUP
TOP_K = 2
D_EXPERT = 512
MOE_BLOCK = 256
EPS = 1e-6

kernel_name = "hybrid_mla_gdn_hmoe_encoder"


def rmsnorm(x, g):
    xf = x.astype(jnp.float32)
    y = xf * lax.rsqrt(jnp.mean(xf * xf, axis=-1, keepdims=True) + EPS) * g.astype(jnp.float32)
    return y.astype(x.dtype)


def l2norm(x):
    return x * lax.rsqrt(jnp.sum(x * x, axis=-1, keepdims=True) + EPS)


def split_points(sizes):
    return [int(v) for v in np.cumsum(sizes)[:-1]]


def rope_tables(seq, dtype):
    inv_freq = ROPE_THETA ** (-jnp.arange(0, QK_ROPE, 2, dtype=jnp.float32) / QK_ROPE)
    ang = jnp.arange(seq, dtype=jnp.float32)[:, None] * inv_freq[None, :]
    return jnp.cos(ang).astype(dtype), jnp.sin(ang).astype(dtype)


def apply_rope(x, cos, sin):
    x1, x2 = jnp.split(x, 2, axis=-1)
    return jnp.concatenate([x1 * cos - x2 * sin, x2 * cos + x1 * sin], axis=-1)


def mla(c_q, c_kv, k_pe, g_q, g_kv, w_uq, w_ukv):
    b, s, _ = c_q.shape
    cos, sin = rope_tables(s, c_q.dtype)
    scale = (QK_NOPE + QK_ROPE) ** -0.5
    q = jnp.einsum('bsr,rn->bsn', rmsnorm(c_q, g_q), w_uq).reshape(b, s, MLA_HEADS, QK_NOPE + QK_ROPE) * scale
    q_nope = q[..., :QK_NOPE]
    q_pe = apply_rope(q[..., QK_NOPE:], cos[:, None, :], sin[:, None, :])
    kv = jnp.einsum('bsr,rn->bsn', rmsnorm(c_kv, g_kv), w_ukv).reshape(b, s, MLA_HEADS, QK_NOPE + V_HEAD)
    k_nope, v = kv[..., :QK_NOPE], kv[..., QK_NOPE:]
    k_pe = apply_rope(k_pe, cos, sin)
    nb = s // Q_BLOCK

    def to_blocks(t):
        return jnp.swapaxes(t.reshape(b, nb, Q_BLOCK, *t.shape[2:]), 0, 1)

    def attend(blk):
        qn, qp = blk
        sc = (jnp.einsum('bqhd,bkhd->bhqk', qn, k_nope, preferred_element_type=jnp.float32)
              + jnp.einsum('bqhr,bkr->bhqk', qp, k_pe, preferred_element_type=jnp.float32))
        p = jax.nn.softmax(sc, axis=-1).astype(v.dtype)
        return jnp.einsum('bhqk,bkhd->bqhd', p, v)

    o = lax.map(attend, (to_blocks(q_nope), to_blocks(q_pe)))
    return jnp.swapaxes(o, 0, 1).reshape(b, s, D_ATTN)


def centred_conv(x, w):
    c = x.shape[-1]
    return lax.conv_general_dilated(
        x, w[:, None, :].astype(x.dtype), window_strides=(1,),
        padding=[(CONV_K // 2, CONV_K // 2)],
        dimension_numbers=('NWC', 'WIO', 'NWC'), feature_group_count=c)


def unit_lower_inverse(lower):
    eye = jnp.eye(CHUNK, dtype=lower.dtype)
    nil = -lower
    inv = eye + nil
    power = nil
    for _ in range(int(math.log2(CHUNK)) - 1):
        power = power @ power
        inv = inv + inv @ power
    return inv


def gated_delta_chunked(q, k, v, g, beta):
    b, s, h, dk = q.shape
    n = s // CHUNK

    def chunks(t):
        return t.reshape(b, n, CHUNK, h, -1).transpose(0, 3, 1, 2, 4)

    q, k, v = chunks(q), chunks(k), chunks(v)
    g = g.reshape(b, n, CHUNK, h).transpose(0, 3, 1, 2)
    beta = beta.reshape(b, n, CHUNK, h).transpose(0, 3, 1, 2)
    gc = jnp.cumsum(g, axis=-1)
    idx = jnp.arange(CHUNK)
    incl = idx[:, None] >= idx[None, :]
    decay = jnp.exp(jnp.where(incl, gc[..., :, None] - gc[..., None, :], -jnp.inf))
    k_beta = k * beta[..., None]
    lower = jnp.where(idx[:, None] > idx[None, :],
                      jnp.einsum('bhnid,bhnjd->bhnij', k_beta, k) * decay, 0.0)
    t_inv = unit_lower_inverse(lower)
    u = t_inv @ (v * beta[..., None])
    w = t_inv @ (k_beta * jnp.exp(gc)[..., None])
    intra = jnp.einsum('bhnid,bhnjd->bhnij', q, k) * decay
    q_dec = q * jnp.exp(gc)[..., None]
    g_last = gc[..., -1]
    k_dec = k * jnp.exp(g_last[..., None] - gc)[..., None]
    xs = tuple(jnp.moveaxis(t, 2, 0) for t in (u, w, intra, q_dec, k_dec, g_last))

    def step(state, xs_i):
        u_i, w_i, a_i, qd_i, kd_i, gl_i = xs_i
        v_new = u_i - w_i @ state
        o_i = qd_i @ state + a_i @ v_new
        state = state * jnp.exp(gl_i)[..., None, None] + jnp.swapaxes(kd_i, -1, -2) @ v_new
        return state, o_i

    state0 = jnp.zeros((b, h, dk, v.shape[-1]), q.dtype)
    _, o = lax.scan(step, state0, xs)
    return o.transpose(1, 0, 3, 2, 4).reshape(b, s, h, -1)


def gdn(qkv, z, a, bt, conv_w, a_log, dt_bias, g_out):
    b, s, _ = qkv.shape
    qkv = jax.nn.silu(centred_conv(qkv, conv_w)).astype(jnp.float32)
    q, k, v = jnp.split(qkv, [GDN_HEADS * GDN_DK, 2 * GDN_HEADS * GDN_DK], axis=-1)
    q = l2norm(q.reshape(b, s, GDN_HEADS, GDN_DK)) * (GDN_DK ** -0.5)
    k = l2norm(k.reshape(b, s, GDN_HEADS, GDN_DK))
    v = v.reshape(b, s, GDN_HEADS, GDN_DV)
    beta = jax.nn.sigmoid(bt.astype(jnp.float32)).reshape(b, s, 2, GDN_HEADS)
    g = -jnp.exp(a_log.astype(jnp.float32)) * jax.nn.softplus(
        a.astype(jnp.float32).reshape(b, s, 2, GDN_HEADS) + dt_bias.astype(jnp.float32))
    o_fwd = gated_delta_chunked(q, k, v, g[:, :, 0], beta[:, :, 0])
    rev = lambda t: jnp.flip(t, axis=1)
    o_bwd = rev(gated_delta_chunked(rev(q), rev(k), rev(v), rev(g[:, :, 1]), rev(beta[:, :, 1])))
    o = rmsnorm(o_fwd + o_bwd, g_out) * jax.nn.silu(z.astype(jnp.float32).reshape(b, s, GDN_HEADS, GDN_DV))
    return o.reshape(b, s, D_GDN).astype(z.dtype)


def hier_moe(x, w_rg, b_rg, w_re, b_re, w_gate, w_up, w_down):
    b, s, d = x.shape
    t = b * s
    xt = x.reshape(t, d)
    group_logits = jnp.einsum('td,dg->tg', xt, w_rg, preferred_element_type=jnp.float32) + b_rg.astype(jnp.float32)
    group_prob = jax.nn.softmax(group_logits, axis=-1)
    grp = jnp.argmax(group_logits, axis=-1).astype(jnp.int32)
    grp_p = jnp.take_along_axis(group_prob, grp[:, None], axis=1)
    exp_logits = (jnp.einsum('td,de->te', xt, w_re, preferred_element_type=jnp.float32)
                  + b_re.astype(jnp.float32)).reshape(t, N_GROUPS, EXPERTS_PER_GROUP)
    in_grp = jnp.take_along_axis(exp_logits, grp[:, None, None], axis=1)[:, 0]
    top_p, top_i = lax.top_k(jax.nn.softmax(in_grp, axis=-1), TOP_K)
    gate = grp_p * top_p / jnp.sum(top_p, axis=-1, keepdims=True)
    eid = (grp[:, None] * EXPERTS_PER_GROUP + top_i.astype(jnp.int32)).reshape(-1)
    m = t * TOP_K
    order = jnp.argsort(eid)
    counts = jnp.zeros((N_EXPERTS,), jnp.int32).at[eid].add(1)
    starts = jnp.cumsum(counts) - counts
    padded = (counts + MOE_BLOCK - 1) // MOE_BLOCK * MOE_BLOCK
    pend = jnp.cumsum(padded)
    pstart = pend - padded
    sorted_e = eid[order]
    dest = pstart[sorted_e] + (jnp.arange(m, dtype=jnp.int32) - starts[sorted_e])
    n_blocks = -(-m // MOE_BLOCK) + N_EXPERTS
    p_rows = n_blocks * MOE_BLOCK
    slot_at = jnp.full((p_rows,), m, jnp.int32).at[dest].set(order.astype(jnp.int32))
    tok_of_slot = jnp.concatenate([jnp.arange(m, dtype=jnp.int32) // TOP_K, jnp.array([t], jnp.int32)])
    x_pad = jnp.concatenate([xt, jnp.zeros((1, d), xt.dtype)], axis=0)
    xb = x_pad[tok_of_slot[slot_at]].reshape(n_blocks, MOE_BLOCK, d)
    block_e = jnp.minimum(jnp.searchsorted(pend, jnp.arange(n_blocks, dtype=jnp.int32) * MOE_BLOCK, side='right'),
                          N_EXPERTS - 1)

    def expert_block(args):
        xb_i, e = args
        hdn = jax.nn.silu(xb_i @ w_gate[e]) * (xb_i @ w_up[e])
        return hdn @ w_down[e]

    yb = lax.map(expert_block, (xb, block_e)).reshape(p_rows, d)
    pos = jnp.zeros((m,), jnp.int32).at[order].set(dest)
    y = jnp.sum(yb[pos].reshape(t, TOP_K, d) * gate[..., None].astype(yb.dtype), axis=1)
    return y.reshape(b, s, d)


def encoder(x, norm_mix, w_in, g_q_lora, g_kv_lora, w_uq, w_ukv, g_attn_out, conv_w, a_log, dt_bias,
            g_gdn_out, w_out, norm_ffn, w_router_group, b_router_group, w_router_expert, b_router_expert,
            w_gate, w_up, w_down, norm_final):
    for l in range(DEPTH):
        n = rmsnorm(x, norm_mix[l])
        proj = jnp.einsum('bsd,dn->bsn', n, w_in[l])
        c_q, c_kv, k_pe, qkv, z, a, bt = jnp.split(proj, split_points(IN_SPLITS), axis=-1)
        attn = rmsnorm(mla(c_q, c_kv, k_pe, g_q_lora[l], g_kv_lora[l], w_uq[l], w_ukv[l]), g_attn_out[l])
        lin = gdn(qkv, z, a, bt, conv_w[l], a_log[l], dt_bias[l], g_gdn_out[l])
        x = x + jnp.einsum('bsm,md->bsd', jnp.concatenate([attn, lin], axis=-1), w_out[l])
        x = x + hier_moe(rmsnorm(x, norm_ffn[l]), w_router_group[l], b_router_group[l], w_router_expert[l],
                         b_router_expert[l], w_gate[l], w_up[l], w_down[l])
    return rmsnorm(x, norm_final)


def setup_inputs(seed: int = 0) -> dict:
    key = jax.random.key(seed)
    ks = jax.random.split(key, 24)
    f32 = jnp.float32
    L = DEPTH

    def nrm(k, shape, scale):
        return jax.random.normal(k, shape, f32) * scale

    def gain(k, shape):
        return 1.0 + 0.02 * jax.random.normal(k, shape, f32)

    dt = jnp.exp(jax.random.uniform(ks[9], (L, 2, GDN_HEADS), f32, math.log(1e-3), math.log(1e-1)))
    return {
        "x_prompt": nrm(ks[0], (BATCH, SEQ, D_MODEL), 1.0),
        "x_sample": nrm(ks[1], (DEC_BATCH, DEC_SEQ, D_MODEL), 1.0),
        "norm_mix": gain(ks[2], (L, D_MODEL)),
        "w_in": nrm(ks[3], (L, D_MODEL, N_IN), D_MODEL ** -0.5),
        "g_q_lora": gain(ks[4], (L, Q_LORA)),
        "g_kv_lora": gain(ks[5], (L, KV_LORA)),
        "w_uq": nrm(ks[6], (L, Q_LORA, MLA_HEADS * (QK_NOPE + QK_ROPE)), Q_LORA ** -0.5),
        "w_ukv": nrm(ks[7], (L, KV_LORA, MLA_HEADS * (QK_NOPE + V_HEAD)), KV_LORA ** -0.5),
        "g_attn_out": gain(ks[8], (L, D_ATTN)),
        "conv_w": nrm(ks[10], (L, CONV_K, GDN_QKV), CONV_K ** -0.5),
        "a_log": jnp.log(jax.random.uniform(ks[11], (L, 2, GDN_HEADS), f32, 1.0, 16.0)),
        "dt_bias": dt + jnp.log(-jnp.expm1(-dt)),
        "g_gdn_out": gain(ks[12], (L, GDN_DV)),
        "w_out": nrm(ks[13], (L, D_MIX, D_MODEL), D_MIX ** -0.5),
        "norm_ffn": gain(ks[14], (L, D_MODEL)),
        "w_router_group": nrm(ks[15], (L, D_MODEL, N_GROUPS), D_MODEL ** -0.5),
        "b_router_group": nrm(ks[16], (L, N_GROUPS), 0.01),
        "w_router_expert": nrm(ks[17], (L, D_MODEL, N_EXPERTS), D_MODEL ** -0.5),
        "b_router_expert": nrm(ks[18], (L, N_EXPERTS), 0.01),
        "w_gate": nrm(ks[19], (L, N_EXPERTS, D_MODEL, D_EXPERT), D_MODEL ** -0.5),
        "w_up": nrm(ks[20], (L, N_EXPERTS, D_MODEL, D_EXPERT), D_MODEL ** -0.5),
        "w_down": nrm(ks[21], (L, N_EXPERTS, D_EXPERT, D_MODEL), D_EXPERT ** -0.5),
        "norm_final": gain(ks[22], (D_MODEL,)),
    }


def reference(x_prompt, x_sample, norm_mix, w_in, g_q_lora, g_kv_lora, w_uq, w_ukv, g_attn_out, conv_w,
              a_log, dt_bias, g_gdn_out, w_out, norm_ffn, w_router_group, b_router_group, w_router_expert,
              b_router_expert, w_gate, w_up, w_down, norm_final):
    params = (norm_mix, w_in, g_q_lora, g_kv_lora, w_uq, w_ukv, g_attn_out, conv_w, a_log, dt_bias,
              g_gdn_out, w_out, norm_ffn, w_router_group, b_router_group, w_router_expert, b_router_expert,
              w_gate, w_up, w_down, norm_final)
    y_prompt = encoder(x_prompt, *params)
    y_sample = encoder(x_sample, *params)
    return (y_prompt, y_sample)
```

```python
import contextlib
import numpy as np
import concourse.bass as bass
import concourse.mybir as mybir
from concourse.bass_utils import run_bass_kernel_spmd

F32 = mybir.dt.float32
BF16 = mybir.dt.bfloat16
I32 = mybir.dt.int32
ALU = mybir.AluOpType
AF = mybir.ActivationFunctionType
AX = mybir.AxisListType

N_DMA_SEMS = 6
D = 2048
H = 8
EPS = 1e-6
NEXP = 64
DEXP = 512


class Res:
    __slots__ = ("lw", "rd", "psum")

    def __init__(self):
        self.lw = []
        self.rd = {}
        self.psum = False


class Op:
    __slots__ = ("eng", "fn", "deps", "idx", "dma", "sem", "val", "need")


class Builder:
    ENGS = ("pe", "act", "dve", "pool", "sp")

    def __init__(self, nc):
        self.nc = nc
        self.nops = 0
        self.streams = {e: [] for e in self.ENGS}
        self.last = {e: None for e in self.ENGS}
        self.phase_dmas = []
        self.bar = {e: [] for e in self.ENGS}

    def op(self, eng, fn, reads=(), writes=(), dma=False, partial=False):
        o = Op()
        o.eng, o.fn, o.dma = eng, fn, dma
        o.idx = self.nops
        self.nops += 1
        o.need = False
        o.sem = None
        o.val = 0
        deps = {}

        def add(d, raw):
            if d.eng == eng and not d.dma:
                if eng == "pe" or not raw:
                    return
            key = ("d", d.idx) if d.dma else d.eng
            old = deps.get(key)
            if old is None or old.idx < d.idx:
                deps[key] = d

        for d in self.bar[eng]:
            add(d, True)
        self.bar[eng] = []
        for r in reads:
            for d in r.lw:
                add(d, True)
            if r.psum:
                for rr in r.rd.values():
                    add(rr, False)
        for w in writes:
            for d in w.lw:
                add(d, False)
            for rr in w.rd.values():
                add(rr, False)
        for r in reads:
            r.rd[("d", o.idx) if dma else eng] = o
        for w in writes:
            if partial:
                w.lw = w.lw + [o]
            else:
                w.lw = [o]
            w.rd = {}
        o.deps = list(deps.values())
        for d in o.deps:
            d.need = True
        self.streams[eng].append(o)
        if dma:
            self.phase_dmas.append(o)
        else:
            self.last[eng] = o
        return o

    def barrier(self):
        deps = [o for o in self.last.values() if o is not None] + self.phase_dmas
        self.phase_dmas = []
        for e in self.ENGS:
            self.bar[e] = list(deps)

    def emit(self, final_waits=()):
        nc = self.nc
        with contextlib.ExitStack() as st:
            esem = {e: st.enter_context(nc.semaphore(f"s_{e}")) for e in self.ENGS}
            dsem = {e: [st.enter_context(nc.semaphore(f"d_{e}{i}")) for i in range(N_DMA_SEMS)]
                    for e in ("sp", "pool", "act")}
            cnt = {e: 0 for e in self.ENGS}
            dcnt = {e: [0] * N_DMA_SEMS for e in dsem}
            drr = {e: 0 for e in dsem}
            for e in self.ENGS:
                for o in self.streams[e]:
                    if o.dma:
                        k = drr[e]
                        drr[e] = (k + 1) % N_DMA_SEMS
                        o.sem = (e, k)
                        dcnt[e][k] += 16
                        o.val = dcnt[e][k]
                    elif o.need:
                        cnt[e] += 1
                        o.val = cnt[e]
            block = st.enter_context(nc.Block())
            engobj = {"pe": "tensor", "act": "scalar", "dve": "vector", "pool": "gpsimd", "sp": "sync"}

            def make(e):
                def body(eng):
                    waited = {}
                    for o in self.streams[e]:
                        for d in o.deps:
                            if d.dma:
                                s = dsem[d.sem[0]][d.sem[1]]
                                key = ("d",) + d.sem
                            else:
                                s = esem[d.eng]
                                key = d.eng
                            if waited.get(key, 0) >= d.val:
                                continue
                            eng.wait_ge(s, d.val)
                            waited[key] = d.val
                        if o.dma:
                            s = dsem[o.sem[0]][o.sem[1]]
                            key = ("d",) + o.sem
                            prev = o.val - 16
                            if prev > 0 and waited.get(key, 0) < prev:
                                eng.wait_ge(s, prev)
                                waited[key] = prev
                            o.fn(eng).then_inc(s, 16)
                        else:
                            ins = o.fn(eng)
                            if o.need:
                                ins.then_inc(esem[e], 1)
                    if e == "sp":
                        done = {}
                        for fo in final_waits:
                            done[fo.sem] = max(done.get(fo.sem, 0), fo.val)
                        for (q, k), v in done.items():
                            eng.wait_ge(dsem[q][k], v)
                return body

            for e in self.ENGS:
                getattr(block, engobj[e])(make(e))


class View:
    __slots__ = ("tile", "ap")

    def __init__(self, tile, ap):
        self.tile, self.ap = tile, ap

    def __getitem__(self, idx):
        return View(self.tile, self.ap[idx])

    def re(self, pat, **kw):
        return View(self.tile, self.ap.rearrange(pat, **kw))


class Tile:
    def __init__(self, t, dram=False):
        self.t = t
        self.r = None if dram else Res()

    def __getitem__(self, idx):
        return View(self, self.t[idx])


class Rot:
    def __init__(self, items):
        self.items = list(items)
        self.i = 0

    def next(self):
        x = self.items[self.i % len(self.items)]
        self.i += 1
        return x


class Prog:
    def __init__(self, S_S, S_P, NoP, debug=False):
        self.S_S, self.S_P, self.NoP = S_S, S_P, NoP
        self.debug = debug
        self.nc = bass.Bass("TRN2", target_bir_lowering=False)
        self.b = Builder(self.nc)
        self.inputs = {}
        self.out_ops = []
        self.dbg_names = []

    def din(self, name, shape, dtype=F32):
        t = Tile(self.nc.dram_tensor(name, list(shape), dtype, kind="ExternalInput").ap(), dram=True)
        self.inputs[name] = t
        return t

    def dscr(self, name, shape, dtype=F32):
        kind = "ExternalOutput" if self.debug else "Internal"
        if self.debug:
            self.dbg_names.append(name)
        return Tile(self.nc.dram_tensor(name, list(shape), dtype, kind=kind).ap(), dram=True)

    def sb(self, st, name, shape, dtype=F32):
        self.uid = getattr(self, "uid", 0) + 1
        return Tile(st.enter_context(self.nc.sbuf_tensor(f"{name}_{self.uid}", list(shape), dtype)))

    def ring(self, st, name, shape, dtype, n):
        return Rot([self.sb(st, f"{name}{i}", shape, dtype) for i in range(n)])

    def mm(self, out, lhsT, rhs, start=True, stop=True):
        self.b.op("pe", lambda e: e.matmul(out.ap, lhsT.ap, rhs.ap, start=start, stop=stop),
                  reads=[lhsT.tile.r, rhs.tile.r], writes=[out.tile.r])

    def tr(self, out, in_, ident):
        self.b.op("pe", lambda e: e.transpose(out.ap, in_.ap, ident.ap),
                  reads=[in_.tile.r, ident.tile.r], writes=[out.tile.r])

    def act(self, out, in_, func, bias=None, scale=None, accum=None):
        rd = [in_.tile.r]
        kw = {}
        if bias is not None:
            if isinstance(bias, View):
                rd.append(bias.tile.r)
                kw["bias"] = bias.ap
            else:
                kw["bias"] = float(bias)
        if scale is not None:
            if isinstance(scale, View):
                rd.append(scale.tile.r)
                kw["scale"] = scale.ap
            else:
                kw["scale"] = float(scale)
        wr = [out.tile.r]
        if accum is not None:
            wr.append(accum.tile.r)
            kw["accum_out"] = accum.ap
        self.b.op("act", lambda e: e.activation(out.ap, in_.ap, func, **kw), reads=rd, writes=wr)

    def cp(self, eng, out, in_):
        if eng == "act":
            self.b.op("act", lambda e: e.copy(out.ap, in_.ap), reads=[in_.tile.r], writes=[out.tile.r])
        else:
            self.b.op(eng, lambda e: e.tensor_copy(out.ap, in_.ap), reads=[in_.tile.r], writes=[out.tile.r])

    def tt(self, eng, out, a, b2, op):
        self.b.op(eng, lambda e: e.tensor_tensor(out.ap, a.ap, b2.ap, op),
                  reads=[a.tile.r, b2.tile.r], writes=[out.tile.r])

    def ts(self, eng, out, a, s1, s2, op0, op1=None):
        rd = [a.tile.r]
        s1v = s1.ap if isinstance(s1, View) else s1
        s2v = s2.ap if isinstance(s2, View) else s2
        if isinstance(s1, View):
            rd.append(s1.tile.r)
        if isinstance(s2, View):
            rd.append(s2.tile.r)
        if op1 is None:
            self.b.op(eng, lambda e: e.tensor_scalar(out.ap, a.ap, s1v, None, op0), reads=rd, writes=[out.tile.r])
        else:
            self.b.op(eng, lambda e: e.tensor_scalar(out.ap, a.ap, s1v, s2v, op0, op1), reads=rd, writes=[out.tile.r])

    def stt(self, eng, out, a, sc, b2, op0, op1):
        assert eng == "dve"
        rd = [a.tile.r, b2.tile.r]
        scv = sc.ap if isinstance(sc, View) else sc
        if isinstance(sc, View):
            rd.append(sc.tile.r)
        self.b.op(eng, lambda e: e.scalar_tensor_tensor(out.ap, a.ap, scv, b2.ap, op0, op1),
                  reads=rd, writes=[out.tile.r])

    def onehot_max(self, out, a, m):
        self.ts("dve", out, a, m, None, ALU.subtract)
        self.ts("dve", out, out, 1e30, 1.0, ALU.mult, ALU.add)
        self.ts("dve", out, out, 0.0, None, ALU.max)

    def recip(self, out, a):
        self.b.op("dve", lambda e: e.reciprocal(out.ap, a.ap), reads=[a.tile.r], writes=[out.tile.r])

    def memset(self, eng, out, val):
        self.b.op(eng, lambda e: e.memset(out.ap, val), writes=[out.tile.r])

    def asel(self, out, in_, cm, pat, cmp, fill):
        self.b.op("pool", lambda e: e.affine_select(out.ap, in_.ap, pattern=[[pat, 128]], compare_op=cmp,
                                                    fill=fill, base=0, channel_multiplier=cm),
                  reads=[in_.tile.r], writes=[out.tile.r])

    def rmax(self, out, a):
        self.b.op("dve", lambda e: e.reduce_max(out.ap, a.ap, AX.X), reads=[a.tile.r], writes=[out.tile.r])

    def rsum(self, out, a):
        self.b.op("dve", lambda e: e.reduce_sum(out.ap, a.ap, AX.X), reads=[a.tile.r], writes=[out.tile.r])

    def dma(self, q, out, in_, partial=False, infn=None, extra_reads=()):
        rd = [r for r in [in_.tile.r] + [x.r for x in extra_reads] if r is not None]
        wr = [r for r in [out.tile.r] if r is not None]
        if infn is None:
            fn = lambda e: e.dma_start(out=out.ap, in_=in_.ap)
        else:
            fn = lambda e: e.dma_start(out=out.ap, in_=infn(e))
        return self.b.op(q, fn, reads=rd, writes=wr, dma=True, partial=partial)

    def build(self):
        nc = self.nc
        S_S, S_P, NoP = self.S_S, self.S_P, self.NoP
        I = self.din
        self.xs = I("xs", [S_S, D])
        self.xp = I("xp", [S_P, D])
        self.xpo = I("xpo", [NoP, D])
        self.w_ctx = I("w_ctx", [D, 3840])
        self.w_own = I("w_own", [D, 1536])
        self.w_uqn = I("w_uqn", [512, 1024])
        self.w_uqp = I("w_uqp", [512, 512])
        self.w_uqps = I("w_uqps", [512, 512])
        self.w_uk = I("w_uk", [512, 1024])
        self.w_uv = I("w_uv", [512, 1024])
        self.w_out = I("w_out", [D, D])
        self.w_r = I("w_r", [D, 72])
        self.b_r = I("b_r", [128, 72])
        self.g_mix = I("g_mix", [128, D])
        self.g_ffn = I("g_ffn", [128, D])
        self.g_fin = I("g_fin", [128, D])
        self.g_q = I("g_q", [128, 4])
        self.g_kv = I("g_kv", [128, 4])
        self.g_att = I("g_att", [128, 8])
        self.g_gdn = I("g_gdn", [128, 128])
        self.conv = I("conv", [128, 24, 5])
        self.alog = I("alog", [128, 16])
        self.dtb = I("dtb", [128, 16])
        self.cs_s = I("cs_s", [64, S_S])
        self.sn_s = I("sn_s", [64, S_S])
        self.cs_p = I("cs_p", [64, S_P])
        self.sn_p = I("sn_p", [64, S_P])
        self.cs_po = I("cs_po", [64, NoP])
        self.sn_po = I("sn_po", [64, NoP])
        self.own_off = I("own_off", [1, 1], I32)
        self.sel4 = I("sel4", [128, 4])
        import os
        self.nexp_in = 1 if "7" in os.environ.get("MK_SKIP", "").split(",") else NEXP
        self.w_gate = I("w_gate", [self.nexp_in, D, DEXP])
        self.w_up = I("w_up", [self.nexp_in, D, DEXP])
        self.w_down = I("w_down", [self.nexp_in, DEXP, D])
        self.ys = Tile(nc.dram_tensor("ys", [S_S, D], F32, kind="ExternalOutput").ap(), dram=True)
        self.yp = Tile(nc.dram_tensor("yp", [NoP, D], F32, kind="ExternalOutput").ap(), dram=True)

        with contextlib.ExitStack() as st0:
            self.consts(st0)
            jobs = [
                dict(n="s", Sc=S_S, No=S_S, xc=self.xs, xo=self.xs, cs=self.cs_s, sn=self.sn_s,
                     cso=self.cs_s, sno=self.sn_s, y=self.ys, dyn=False),
                dict(n="p", Sc=S_P, No=NoP, xc=self.xp, xo=self.xpo, cs=self.cs_p, sn=self.sn_p,
                     cso=self.cs_po, sno=self.sn_po, y=self.yp, dyn=True),
            ]
            import os
            self.skip = set(int(x) for x in os.environ.get("MK_SKIP", "").split(",") if x)
            jsel = os.environ.get("MK_JOBS", "sp")
            for J in jobs:
                if J["n"] in jsel:
                    self.job(J)
            self.b.barrier()
        self.b.emit(final_waits=self.out_ops)
        return nc

    def consts(self, st):
        sb = self.sb
        nc = self.nc
        self.ps = [Tile(nc.alloc_psum_tensor(f"ps{i}", [128, 512], F32)) for i in range(6)]
        self.pb = [Tile(nc.alloc_psum_tensor(f"pb{i}", [128, 1024], BF16)) for i in range(2)]
        for t in self.ps + self.pb:
            t.r.psum = True
        self.idf = sb(st, "idf", [128, 128])
        self.idb = sb(st, "idb", [128, 128], BF16)
        self.onef = sb(st, "onef", [128, 128])
        self.oneb = sb(st, "oneb", [128, 128], BF16)
        self.zero = sb(st, "zero", [128, 128])
        self.memset("pool", self.zero[:], 0.0)
        self.memset("pool", self.onef[:], 1.0)
        self.cp("dve", self.oneb[:], self.onef[:])
        self.asel(self.idf[:], self.zero[:], 1, -1, ALU.not_equal, 1.0)
        self.cp("dve", self.idb[:], self.idf[:])
        self.minclT, self.nmT, self.pms = [], [], []
        for d in range(2):
            a = (-1, 1) if d == 0 else (1, -1)
            c = (1, -1) if d == 0 else (-1, 1)
            m = sb(st, f"minclT{d}", [128, 128])
            self.asel(m[:], self.onef[:], a[0], a[1], ALU.is_ge, 0.0)
            n = sb(st, f"nmT{d}", [128, 128])
            self.asel(n[:], self.zero[:], a[0], a[1], ALU.is_ge, -30000.0)
            p = sb(st, f"pms{d}", [128, 128])
            self.asel(p[:], self.zero[:], c[0], c[1], ALU.is_gt, 30000.0)
            self.minclT.append(m)
            self.nmT.append(n)
            self.pms.append(p)
        def ld(name, src, shape):
            t = sb(st, name, shape)
            self.dma("sp", t[:], src[:])
            return t
        self.gq = ld("gq", self.g_q, [128, 4])
        self.gkv = ld("gkv", self.g_kv, [128, 4])
        self.gatt = ld("gatt", self.g_att, [128, 8])
        self.ggdn = ld("ggdn", self.g_gdn, [128, 128])
        self.cw = ld("cw", self.conv, [128, 24, 5])
        self.br = ld("br", self.b_r, [128, 72])
        al = ld("al", self.alog, [128, 16])
        self.dtbias = ld("dtbias", self.dtb, [128, 16])
        self.nea = sb(st, "nea", [128, 16])
        self.act(self.nea[:], al[:], AF.Exp)
        self.ts("dve", self.nea[:], self.nea[:], -1.0, None, ALU.mult)

    def ldw(self, st, name, src, ncol):
        t = self.sb(st, name, [128, 4, ncol], BF16)
        self.dma("pool", t[:], src[:, :].re("(c p) n -> p c n", p=128))
        return t

    def norm_T(self, st_tiles, xrows, grep, nT_dst):
        xt_r, junk, ss_r, xn_r = st_tiles
        xt = xt_r.next()
        self.dma("sp", xt[:], xrows)
        ss = ss_r.next()
        self.act(junk[:], xt[:], AF.Square, accum=ss[:, 0:1])
        self.ts("dve", ss[:, 1:2], ss[:, 0:1], 1.0 / D, EPS, ALU.mult, ALU.add)
        self.act(ss[:, 2:3], ss[:, 1:2], AF.Sqrt)
        self.recip(ss[:, 3:4], ss[:, 2:3])
        xn = xn_r.next()
        self.stt("dve", xn[:], xt[:], ss[:, 3:4], grep[:], ALU.mult, ALU.mult)
        for hb in range(2):
            pb = self.pb[hb]
            for c in range(8):
                cc = hb * 8 + c
                self.tr(pb[:, c * 128:(c + 1) * 128], xn[:, cc * 128:(cc + 1) * 128], self.idb[:])
            self.cp("act" if hb == 0 else "dve", nT_dst[:, hb * 8:(hb + 1) * 8, :],
                    pb[:, :].re("p (c n) -> p c n", c=8))
        return xt

    def fm_norm(self, raw, nch, dim, gain, out, sq, rb, psbank):
        for c in range(nch):
            self.act(sq[:, c, :], raw[:, c, :], AF.Square)
        for c in range(nch):
            self.mm(psbank[:, :], self.oneb[:], sq[:, c, :], start=(c == 0), stop=(c == nch - 1))
        self.act(rb[:], psbank[:, :], AF.Sqrt, bias=EPS, scale=1.0 / dim)
        self.recip(rb[:], rb[:])
        for c in range(nch):
            self.stt("dve", out[:, c, :], raw[:, c, :], gain[:, c:c + 1], rb[:],
                     ALU.mult, ALU.mult)

    def job(self, J):
        n, Sc, No = J["n"], J["Sc"], J["No"]
        d = self.dscr
        J["KT"] = d(f"KT_{n}", [H, 128, Sc], BF16)
        J["KPE"] = d(f"KPE_{n}", [64, Sc], BF16)
        J["V"] = d(f"V_{n}", [Sc, 1024], BF16)
        J["PRE"] = d(f"PRE_{n}", [24, 128, Sc + 4], F32)
        J["GB"] = d(f"GB_{n}", [Sc, 32], F32)
        J["QTg"] = d(f"QTg_{n}", [H, 128, Sc], BF16)
        J["KTg"] = d(f"KTg_{n}", [H, 128, Sc], BF16)
        J["Ktm"] = d(f"Ktm_{n}", [Sc, 1024], BF16)
        J["Vtm"] = d(f"Vtm_{n}", [Sc, 1024], BF16)
        J["OF"] = d(f"OF_{n}", [Sc, 1024], F32)
        J["OS"] = d(f"OS_{n}", [Sc, 1024], F32)
        J["QN"] = d(f"QN_{n}", [H, 128, No], BF16)
        J["QP"] = d(f"QP_{n}", [H, 64, No], BF16)
        J["SZ"] = d(f"SZ_{n}", [No, 1024], F32)
        J["ATT"] = d(f"ATT_{n}", [H, 128, No], F32)
        J["X1"] = d(f"X1_{n}", [No, D], F32)
        J["XN2"] = d(f"XN2_{n}", [16, 128, No], BF16)
        J["GT"] = d(f"GT_{n}", [NEXP, No], F32)
        phs = [lambda: self.phaseA(J), lambda: self.phaseA2(J), lambda: self.phaseB(J, 0), lambda: self.phaseB(J, 1),
               lambda: self.phaseC(J), lambda: self.phaseD(J), lambda: self.phaseE(J), lambda: self.phaseF(J)]
        for i, ph in enumerate(phs):
            if i in self.skip:
                continue
            ph()
            self.b.barrier()

    def phaseA(self, J):
        Sc = J["Sc"]
        with contextlib.ExitStack() as st:
            sb, ring = self.sb, self.ring
            grep = sb(st, "A_g", [128, D])
            self.dma("sp", grep[:], self.g_mix[:])
            self.wuk = self.ldw(st, "wuk", self.w_uk, 1024)
            self.wuv = self.ldw(st, "wuv", self.w_uv, 1024)
            ntl = (ring(st, "A_xt", [128, D], F32, 2), sb(st, "A_junk", [128, D], BF16),
                   ring(st, "A_ss", [128, 4], F32, 2), ring(st, "A_xn", [128, D], BF16, 2))
            nT = ring(st, "A_nT", [128, 16, 512], BF16, 2)
            wblk = ring(st, "A_w", [128, 16, 512], BF16, 2)
            raw = sb(st, "A_raw", [128, 4, 512])
            sq = sb(st, "A_sq", [128, 4, 512], BF16)
            rb = sb(st, "A_rb", [128, 512])
            ckn = sb(st, "A_ckn", [128, 4, 512], BF16)
            ev = ring(st, "A_ev", [128, 512], F32, 3)
            evb = ring(st, "A_evb", [128, 512], BF16, 3)
            vsb = ring(st, "A_v", [128, 1024], BF16, 2)
            cst = ring(st, "A_cs", [64, 512], F32, 2)
            snt = ring(st, "A_sn", [64, 512], F32, 2)
            r1 = sb(st, "A_r1", [64, 512])
            r2 = sb(st, "A_r2", [64, 512])
            gbt = ring(st, "A_gb", [128, 32], F32, 2)
            gtmp = ring(st, "A_gtmp", [128, 64], F32, 2)
            zpad = sb(st, "A_zp", [128, 24, 2])
            self.memset("pool", zpad[:], 0.0)
            PRE = J["PRE"]
            self.dma("pool", PRE[:, :, 0:2].re("c p t -> p c t"), zpad[:])
            self.dma("pool", PRE[:, :, Sc + 2:Sc + 4].re("c p t -> p c t"), zpad[:])
            psr = Rot(self.ps[0:5])
            eng2 = Rot(["act", "dve"])
            wv = self.w_ctx[:, :].re("(c p) n -> p c n", p=128)
            for ti in range(Sc // 512):
                t0 = ti * 512
                nTt = nT.next()
                for s in range(4):
                    self.norm_T(ntl, J["xc"][t0 + s * 128:t0 + (s + 1) * 128, :], grep,
                                nTt[:, :, s * 128:(s + 1) * 128])
                wb = wblk.next()
                self.dma("pool", wb[:], wv[:, :, 0:512])
                for j in range(4):
                    p = psr.next()
                    for c in range(16):
                        self.mm(p[:, :], wb[:, c, j * 128:(j + 1) * 128], nTt[:, c, :], start=(c == 0), stop=(c == 15))
                    self.cp(eng2.next(), raw[:, j, :], p[:, :])
                self.fm_norm(raw, 4, 512, self.gkv, ckn, sq, rb, self.ps[5])
                for h in range(H):
                    p = psr.next()
                    for c in range(4):
                        self.mm(p[:, :], self.wuk[:, c, h * 128:(h + 1) * 128], ckn[:, c, :], start=(c == 0), stop=(c == 3))
                    e = evb.next()
                    self.cp(eng2.next(), e[:], p[:, :])
                    self.dma("pool", J["KT"][h, :, t0:t0 + 512], e[:])
                for s in range(4):
                    v = vsb.next()
                    for hb in range(2):
                        p = psr.next()
                        for c in range(4):
                            self.mm(p[:, :], ckn[:, c, s * 128:(s + 1) * 128], self.wuv[:, c, hb * 512:(hb + 1) * 512],
                                    start=(c == 0), stop=(c == 3))
                        self.cp(eng2.next(), v[:, hb * 512:(hb + 1) * 512], p[:, :])
                    self.dma("pool", J["V"][t0 + s * 128:t0 + (s + 1) * 128, :], v[:])
                for blk in range(6):
                    wb = wblk.next()
                    self.dma("pool", wb[:], wv[:, :, 512 + blk * 512:1024 + blk * 512])
                    for j in range(4):
                        p = psr.next()
                        for c in range(16):
                            self.mm(p[:, :], wb[:, c, j * 128:(j + 1) * 128], nTt[:, c, :], start=(c == 0), stop=(c == 15))
                        e = ev.next()
                        self.cp(eng2.next(), e[:], p[:, :])
                        self.dma("pool", PRE[blk * 4 + j, :, 2 + t0:2 + t0 + 512], e[:])
                wb = wblk.next()
                self.dma("pool", wb[:, :, 0:256], wv[:, :, 3584:3840])
                p1 = psr.next()
                for c in range(16):
                    self.mm(p1[0:64, :], wb[:, c, 0:64], nTt[:, c, :], start=(c == 0), stop=(c == 15))
                p2 = psr.next()
                for c in range(16):
                    self.mm(p2[0:64, :], wb[:, c, 64:128], nTt[:, c, :], start=(c == 0), stop=(c == 15))
                cs, sn = cst.next(), snt.next()
                self.dma("sp", cs[:], J["cs"][:, t0:t0 + 512])
                self.dma("sp", sn[:], J["sn"][:, t0:t0 + 512])
                self.tt("dve", r1[:], p1[0:64, :], cs[:], ALU.mult)
                self.tt("dve", r2[:], p2[0:64, :], sn[:], ALU.mult)
                e = evb.next()
                self.tt("pool", e[0:64, :], r1[:], r2[:], ALU.add)
                self.dma("pool", J["KPE"][:, t0:t0 + 512], e[0:64, :])
                for s in range(4):
                    p = psr.next()
                    for c in range(16):
                        self.mm(p[:, 0:32], nTt[:, c, s * 128:(s + 1) * 128], wb[:, c, 128:160], start=(c == 0), stop=(c == 15))
                    gb, g = gbt.next(), gtmp.next()
                    self.tt("dve", g[:, 0:16], p[:, 0:16], self.dtbias[:], ALU.add)
                    self.ts("dve", g[:, 48:64], g[:, 0:16], -1.0, None, ALU.mult)
                    self.tt("dve", g[:, 16:32], g[:, 0:16], g[:, 48:64], ALU.max)
                    self.act(g[:, 16:32], g[:, 16:32], AF.Exp, scale=-1.0)
                    self.act(g[:, 16:32], g[:, 16:32], AF.Ln, bias=1.0)
                    self.ts("dve", g[:, 32:48], g[:, 0:16], 0.0, None, ALU.max)
                    self.tt("dve", g[:, 32:48], g[:, 32:48], g[:, 16:32], ALU.add)
                    self.tt("dve", gb[:, 0:16], g[:, 32:48], self.nea[:], ALU.mult)
                    self.act(gb[:, 16:32], p[:, 16:32], AF.Sigmoid)
                    self.dma("sp", J["GB"][t0 + s * 128:t0 + (s + 1) * 128, :], gb[:])

    def phaseA2(self, J):
        Sc = J["Sc"]
        with contextlib.ExitStack() as st:
            sb, ring = self.sb, self.ring
            x3 = ring(st, "A2_x", [128, 3, 516], F32, 2)
            y3 = ring(st, "A2_y", [128, 3, 512], F32, 2)
            sq = ring(st, "A2_sq", [128, 2, 512], BF16, 2)
            rb = ring(st, "A2_rb", [128, 2, 512], F32, 2)
            qk = ring(st, "A2_qk", [128, 2, 512], BF16, 2)
            vb = ring(st, "A2_vb", [128, 512], BF16, 2)
            tm = ring(st, "A2_tm", [128, 2, 4, 128], BF16, 2)
            PRE = J["PRE"]
            psr = Rot(self.ps[0:4])
            for ti in range(Sc // 512):
                t0 = ti * 512
                for h in range(H):
                    x = x3.next()
                    self.dma("sp", x[:], PRE[h::8, :, t0:t0 + 516].re("c p t -> p c t"))
                    y = y3.next()
                    for c in range(3):
                        ch = c * 8 + h
                        eng = "dve"
                        self.ts(eng, y[:, c, :], x[:, c, 0:512], self.cw[:, ch, 0:1], None, ALU.mult)
                        for i in range(1, 5):
                            self.stt(eng, y[:, c, :], x[:, c, i:i + 512], self.cw[:, ch, i:i + 1], y[:, c, :],
                                     ALU.mult, ALU.add)
                    self.act(y[:], y[:], AF.Silu)
                    s2, r2, q2 = sq.next(), rb.next(), qk.next()
                    self.act(s2[:], y[:, 0:2, :], AF.Square)
                    for c in range(2):
                        p = psr.next()
                        self.mm(p[:, :], self.oneb[:], s2[:, c, :])
                        self.act(r2[:, c, :], p[:, :], AF.Sqrt, bias=EPS)
                    self.recip(r2[:], r2[:])
                    self.stt("dve", q2[:, 0, :], y[:, 0, :], 128.0 ** -0.5, r2[:, 0, :], ALU.mult, ALU.mult)
                    self.tt("pool", q2[:, 1, :], y[:, 1, :], r2[:, 1, :], ALU.mult)
                    v = vb.next()
                    self.cp("pool", v[:], y[:, 2, :])
                    self.dma("pool", J["QTg"][h, :, t0:t0 + 512], q2[:, 0, :])
                    self.dma("pool", J["KTg"][h, :, t0:t0 + 512], q2[:, 1, :])
                    pbk, pbv = self.pb[0], self.pb[1]
                    for s in range(4):
                        self.tr(pbk[:, s * 128:(s + 1) * 128], q2[:, 1, s * 128:(s + 1) * 128], self.idb[:])
                    for s in range(4):
                        self.tr(pbv[:, s * 128:(s + 1) * 128], v[:, s * 128:(s + 1) * 128], self.idb[:])
                    t = tm.next()
                    self.cp("act", t[:, 0, :, :], pbk[:, 0:512].re("p (s n) -> p s n", s=4))
                    self.cp("dve", t[:, 1, :, :], pbv[:, 0:512].re("p (s n) -> p s n", s=4))
                    self.dma("sp", J["Ktm"][t0:t0 + 512, h * 128:(h + 1) * 128].re("(s p) n -> p s n", p=128), t[:, 0, :, :])
                    self.dma("sp", J["Vtm"][t0:t0 + 512, h * 128:(h + 1) * 128].re("(s p) n -> p s n", p=128), t[:, 1, :, :])

    def phaseB(self, J, dr):
        Sc = J["Sc"]
        NCH = Sc // 128
        with contextlib.ExitStack() as st:
            sb, ring = self.sb, self.ring
            Sf = [sb(st, f"B_Sf{h}", [128, 128]) for h in range(H)]
            Sb = [sb(st, f"B_Sb{h}", [128, 128], BF16) for h in range(H)]
            for h in range(H):
                self.memset("pool", Sf[h][:], 0.0)
                self.memset("pool", Sb[h][:], 0.0)
            gbr = ring(st, "B_gb", [128, 32], F32, 2)
            cr = ring(st, "B_c", [128, 80], F32, 2)
            qtr = ring(st, "B_qt", [128, 128], BF16, 3)
            ktr = ring(st, "B_kt", [128, 128], BF16, 3)
            kmr = ring(st, "B_km", [128, 128], BF16, 3)
            vmr = ring(st, "B_vm", [128, 128], BF16, 3)
            ofr = ring(st, "B_of", [128, 128], F32, 3)
            dgr = ring(st, "B_dg", [128, 128], F32, 2)
            rm1 = ring(st, "B_rm1", [128, 128], F32, 2)
            rm2 = ring(st, "B_rm2", [128, 128], F32, 2)
            dtr = ring(st, "B_dt", [128, 128], F32, 2)
            dsr = ring(st, "B_ds", [128, 128], F32, 2)
            pr = ring(st, "B_p", [128, 128], BF16, 4)
            ptr = ring(st, "B_pt", [128, 128], BF16, 4)
            xr = ring(st, "B_x", [128, 128], BF16, 4)
            vbr = ring(st, "B_vb", [128, 128], BF16, 2)
            kbr = ring(st, "B_kb", [128, 128], BF16, 2)
            kdr = ring(st, "B_kd", [128, 128], BF16, 2)
            ur = ring(st, "B_u", [128, 128], F32, 2)
            wtr = ring(st, "B_wt", [128, 128], BF16, 2)
            atr = ring(st, "B_at", [128, 128], BF16, 2)
            vnr = ring(st, "B_vn", [128, 128], BF16, 2)
            o1r = ring(st, "B_o1", [128, 128], F32, 2)
            o2r = ring(st, "B_o2", [128, 128], F32, 3)
            psr = Rot(self.ps[0:6])
            ev = Rot(["act", "dve"])
            order = range(NCH) if dr == 0 else range(NCH - 1, -1, -1)
            OUT = J["OF"] if dr == 0 else J["OS"]
            for ci in order:
                t0 = ci * 128
                gb = gbr.next()
                self.dma("sp", gb[:], J["GB"][t0:t0 + 128, :])
                c = cr.next()
                G8 = gb[:, dr * 8:dr * 8 + 8]
                B8 = gb[:, 16 + dr * 8:16 + dr * 8 + 8]
                p = psr.next()
                self.mm(p[:, 0:8], self.minclT[dr][:], G8)
                self.mm(p[:, 8:16], self.onef[:], G8)
                self.cp("dve", c[:, 0:16], p[:, 0:16])
                self.act(c[:, 16:24], c[:, 0:8], AF.Exp)
                self.tt("dve", c[:, 24:32], c[:, 8:16], c[:, 0:8], ALU.subtract)
                self.act(c[:, 24:32], c[:, 24:32], AF.Exp)
                self.act(c[:, 32:40], c[:, 8:16], AF.Exp)
                self.ts("dve", c[:, 40:48], c[:, 0:8], -1.0, None, ALU.mult)
                self.ts("dve", c[:, 48:56], B8, -1.0, None, ALU.mult)
                self.tt("dve", c[:, 56:64], B8, c[:, 16:24], ALU.mult)
                self.tt("dve", c[:, 64:72], B8, c[:, 24:32], ALU.mult)
                for h in range(H):
                    qt, kt, km, vm = qtr.next(), ktr.next(), kmr.next(), vmr.next()
                    self.dma("sp", qt[:], J["QTg"][h, :, t0:t0 + 128])
                    self.dma("sp", kt[:], J["KTg"][h, :, t0:t0 + 128])
                    self.dma("sp", km[:], J["Ktm"][t0:t0 + 128, h * 128:(h + 1) * 128])
                    self.dma("sp", vm[:], J["Vtm"][t0:t0 + 128, h * 128:(h + 1) * 128])
                    if dr == 1:
                        of = ofr.next()
                        self.dma("sp", of[:], J["OF"][t0:t0 + 128, h * 128:(h + 1) * 128])
                    gc, eg, ekd, egl = c[:, h:h + 1], c[:, 16 + h:17 + h], c[:, 24 + h:25 + h], c[:, 32 + h:33 + h]
                    ngc, nb, beg = c[:, 40 + h:41 + h], c[:, 48 + h:49 + h], c[:, 56 + h:57 + h]
                    beta = gb[:, 16 + dr * 8 + h:17 + dr * 8 + h]
                    dg = dgr.next()
                    self.ts("pool", dg[:], self.idf[:], gc, None, ALU.mult)
                    pR = psr.next()
                    self.mm(pR[:, 0:128], self.onef[:], dg[:])
                    m1, m2 = rm1.next(), rm2.next()
                    self.tt("dve", m1[:], pR[:, 0:128], self.nmT[dr][:], ALU.add)
                    self.tt("dve", m2[:], pR[:, 0:128], self.pms[dr][:], ALU.add)
                    DT, Ds = dtr.next(), dsr.next()
                    self.act(DT[:], m1[:], AF.Exp, bias=ngc, scale=1.0)
                    self.act(Ds[:], m2[:], AF.Exp, bias=gc, scale=-1.0)
                    vbt, kbt, kdt = vbr.next(), kbr.next(), kdr.next()
                    self.ts("pool", vbt[:], vm[:], beta, None, ALU.mult)
                    self.ts("pool", kbt[:], km[:], beg, None, ALU.mult)
                    self.ts("pool", kdt[:], km[:], ekd, None, ALU.mult)
                    pG = psr.next()
                    self.mm(pG[:, 0:128], kt[:], kt[:])
                    P = pr.next()
                    self.stt("dve", P[:], pG[:, 0:128], nb, Ds[:], ALU.mult, ALU.mult)
                    pbT = self.pb[h % 2]
                    self.tr(pbT[:, 0:128], P[:], self.idb[:])
                    PT = ptr.next()
                    self.cp("act", PT[:], pbT[:, 0:128])
                    X = xr.next()
                    self.tt("dve", X[:], pbT[:, 0:128], self.idb[:], ALU.add)
                    for k in range(6):
                        pP = psr.next()
                        self.mm(pP[:, 0:128], PT[:], P[:])
                        if k < 5:
                            pPT = psr.next()
                            self.mm(pPT[:, 0:128], P[:], PT[:])
                        P2 = pr.next()
                        self.cp("act", P2[:], pP[:, 0:128])
                        if k < 5:
                            PT2 = ptr.next()
                            self.cp("dve", PT2[:], pPT[:, 0:128])
                        pX = psr.next()
                        self.mm(pX[:, 0:128], P2[:], X[:])
                        X2 = xr.next()
                        self.tt("dve", X2[:], pX[:, 0:128], X[:], ALU.add)
                        P, X = P2, X2
                        if k < 5:
                            PT = PT2
                    pU = psr.next()
                    self.mm(pU[:, 0:128], X[:], vbt[:])
                    U = ur.next()
                    self.cp("act", U[:], pU[:, 0:128])
                    pW = psr.next()
                    self.mm(pW[:, 0:128], kbt[:], X[:])
                    WT = wtr.next()
                    self.cp("act", WT[:], pW[:, 0:128])
                    pA = psr.next()
                    self.mm(pA[:, 0:128], kt[:], qt[:])
                    AT = atr.next()
                    self.tt("dve", AT[:], pA[:, 0:128], DT[:], ALU.mult)
                    pWS = psr.next()
                    self.mm(pWS[:, 0:128], WT[:], Sb[h][:])
                    VN = vnr.next()
                    self.tt("dve", VN[:], U[:], pWS[:, 0:128], ALU.subtract)
                    pQ = psr.next()
                    self.mm(pQ[:, 0:128], qt[:], Sb[h][:])
                    o1 = o1r.next()
                    self.ts("dve", o1[:], pQ[:, 0:128], eg, None, ALU.mult)
                    pAV = psr.next()
                    self.mm(pAV[:, 0:128], AT[:], VN[:])
                    o2 = o2r.next()
                    self.tt("dve", o2[:], pAV[:, 0:128], o1[:], ALU.add)
                    if dr == 1:
                        self.tt("pool", o2[:], o2[:], of[:], ALU.add)
                    self.dma("pool", OUT[t0:t0 + 128, h * 128:(h + 1) * 128], o2[:])
                    pK = psr.next()
                    self.mm(pK[:, 0:128], kdt[:], VN[:])
                    self.stt("dve", Sf[h][:], Sf[h][:], egl, pK[:, 0:128], ALU.mult, ALU.add)
                    self.cp("act", Sb[h][:], Sf[h][:])

    def phaseC(self, J):
        No = J["No"]
        scale = 192.0 ** -0.5
        with contextlib.ExitStack() as st:
            sb, ring = self.sb, self.ring
            grep = sb(st, "C_g", [128, D])
            self.dma("sp", grep[:], self.g_mix[:])
            self.wuqn = self.ldw(st, "wuqn", self.w_uqn, 1024)
            self.wuqp = self.ldw(st, "wuqp", self.w_uqp, 512)
            self.wuqps = self.ldw(st, "wuqps", self.w_uqps, 512)
            ntl = (ring(st, "C_xt", [128, D], F32, 2), sb(st, "C_junk", [128, D], BF16),
                   ring(st, "C_ss", [128, 4], F32, 2), ring(st, "C_xn", [128, D], BF16, 2))
            nT = ring(st, "C_nT", [128, 16, 512], BF16, 2)
            wblk = ring(st, "C_w", [128, 16, 512], BF16, 2)
            raw = sb(st, "C_raw", [128, 4, 512])
            sq = sb(st, "C_sq", [128, 4, 512], BF16)
            rb = sb(st, "C_rb", [128, 512])
            cqn = sb(st, "C_cqn", [128, 4, 512], BF16)
            evb = ring(st, "C_evb", [128, 512], BF16, 3)
            cst = ring(st, "C_cs", [64, 512], F32, 2)
            snt = ring(st, "C_sn", [64, 512], F32, 2)
            r1 = ring(st, "C_r1", [64, 512], F32, 2)
            r2 = ring(st, "C_r2", [64, 512], F32, 2)
            zs = ring(st, "C_z", [128, 1024], F32, 2)
            psr = Rot(self.ps[0:5])
            eng2 = Rot(["act", "dve"])
            wv = self.w_own[:, :].re("(c p) n -> p c n", p=128)
            for ti in range(No // 512):
                t0 = ti * 512
                nTt = nT.next()
                for s in range(4):
                    self.norm_T(ntl, J["xo"][t0 + s * 128:t0 + (s + 1) * 128, :], grep,
                                nTt[:, :, s * 128:(s + 1) * 128])
                wb = wblk.next()
                self.dma("pool", wb[:], wv[:, :, 0:512])
                for j in range(4):
                    p = psr.next()
                    for c in range(16):
                        self.mm(p[:, :], wb[:, c, j * 128:(j + 1) * 128], nTt[:, c, :], start=(c == 0), stop=(c == 15))
                    self.cp(eng2.next(), raw[:, j, :], p[:, :])
                self.fm_norm(raw, 4, 512, self.gq, cqn, sq, rb, self.ps[5])
                cs, sn = cst.next(), snt.next()
                self.dma("sp", cs[:], J["cso"][:, t0:t0 + 512])
                self.dma("sp", sn[:], J["sno"][:, t0:t0 + 512])
                for h in range(H):
                    p = psr.next()
                    for c in range(4):
                        self.mm(p[:, :], self.wuqn[:, c, h * 128:(h + 1) * 128], cqn[:, c, :], start=(c == 0), stop=(c == 3))
                    e = evb.next()
                    self.act(e[:], p[:, :], AF.Copy, scale=scale)
                    self.dma("pool", J["QN"][h, :, t0:t0 + 512], e[:])
                    p1 = psr.next()
                    for c in range(4):
                        self.mm(p1[0:64, :], self.wuqp[:, c, h * 64:(h + 1) * 64], cqn[:, c, :], start=(c == 0), stop=(c == 3))
                    p2 = psr.next()
                    for c in range(4):
                        self.mm(p2[0:64, :], self.wuqps[:, c, h * 64:(h + 1) * 64], cqn[:, c, :], start=(c == 0), stop=(c == 3))
                    a1, a2 = r1.next(), r2.next()
                    self.tt("dve", a1[:], p1[0:64, :], cs[:], ALU.mult)
                    self.tt("dve", a2[:], p2[0:64, :], sn[:], ALU.mult)
                    e = evb.next()
                    self.tt("pool", a1[:], a1[:], a2[:], ALU.add)
                    self.ts("pool", e[0:64, :], a1[:], scale, None, ALU.mult)
                    self.dma("pool", J["QP"][h, :, t0:t0 + 512], e[0:64, :])
                for blk in range(2):
                    wb = wblk.next()
                    self.dma("pool", wb[:], wv[:, :, 512 + blk * 512:1024 + blk * 512])
                    for s in range(4):
                        p = psr.next()
                        for c in range(16):
                            self.mm(p[:, :], nTt[:, c, s * 128:(s + 1) * 128], wb[:, c, :], start=(c == 0), stop=(c == 15))
                        z = zs.next()
                        self.act(z[:, 0:512], p[:, :], AF.Silu)
                        self.dma("pool", J["SZ"][t0 + s * 128:t0 + (s + 1) * 128, blk * 512:(blk + 1) * 512], z[:, 0:512])

    def phaseD(self, J):
        Sc, No = J["Sc"], J["No"]
        NK = Sc // 128
        with contextlib.ExitStack() as st:
            sb, ring = self.sb, self.ring
            kpe = sb(st, "D_kpe", [64, Sc], BF16)
            self.dma("sp", kpe[:], J["KPE"][:, :])
            ktb = sb(st, "D_kt", [128, Sc], BF16)
            vtb = sb(st, "D_v", [128, NK, 128], BF16)
            qn = ring(st, "D_qn", [128, 512], BF16, 2)
            qp = ring(st, "D_qp", [64, 512], BF16, 2)
            pt = ring(st, "D_pt", [128, 512], BF16, 3)
            rl = ring(st, "D_rl", [128, 512], F32, 2)
            ot = ring(st, "D_ot", [128, 512], F32, 2)
            for h in range(H):
                self.dma("sp", ktb[:], J["KT"][h, :, :])
                self.dma("sp", vtb[:], J["V"][:, h * 128:(h + 1) * 128].re("(t p) n -> p t n", p=128))
                for qi in range(No // 512):
                    q0 = qi * 512
                    a, bq = qn.next(), qp.next()
                    self.dma("sp", a[:], J["QN"][h, :, q0:q0 + 512])
                    self.dma("sp", bq[:], J["QP"][h, :, q0:q0 + 512])
                    par = (h * (No // 512) + qi) % 2
                    pO, pL = self.ps[2 + par], self.ps[4 + par]
                    for kt in range(NK):
                        pS = self.ps[kt % 2]
                        self.mm(pS[:, :], ktb[:, kt * 128:(kt + 1) * 128], a[:], start=True, stop=False)
                        self.mm(pS[:, :], kpe[:, kt * 128:(kt + 1) * 128], bq[:], start=False, stop=True)
                        P = pt.next()
                        self.act(P[:], pS[:, :], AF.Exp)
                        self.mm(pO[:, :], vtb[:, kt, :], P[:], start=(kt == 0), stop=(kt == NK - 1))
                        self.mm(pL[:, :], self.oneb[:], P[:], start=(kt == 0), stop=(kt == NK - 1))
                    r = rl.next()
                    self.recip(r[:], pL[:, :])
                    o = ot.next()
                    self.tt("dve", o[:], pO[:, :], r[:], ALU.mult)
                    self.dma("pool", J["ATT"][h, :, q0:q0 + 512], o[:])

    def phaseE(self, J):
        No = J["No"]
        nc = self.nc
        with contextlib.ExitStack() as st:
            sb, ring = self.sb, self.ring
            wout = sb(st, "E_wout", [128, 16, D], BF16)
            self.dma("pool", wout[:], self.w_out[:, :].re("(c p) n -> p c n", p=128))
            wr = sb(st, "E_wr", [128, 16, 72])
            self.dma("sp", wr[:], self.w_r[:, :].re("(c p) n -> p c n", p=128))
            gffn = sb(st, "E_g", [128, D])
            self.dma("sp", gffn[:], self.g_ffn[:])
            att = ring(st, "E_att", [128, 8, 512], F32, 1)
            sq = sb(st, "E_sq", [128, 8, 512], BF16)
            rb = sb(st, "E_rb", [128, 512])
            mixT = sb(st, "E_mixT", [128, 16, 512], BF16)
            osr = ring(st, "E_os", [128, 1024], F32, 1)
            szr = ring(st, "E_sz", [128, 1024], F32, 1)
            st8 = ring(st, "E_st", [128, 32], F32, 2)
            lin = ring(st, "E_lin", [128, 1024], BF16, 2)
            xo = ring(st, "E_xo", [128, D], F32, 1)
            x1 = ring(st, "E_x1", [128, D], F32, 1)
            junk = sb(st, "E_junk", [128, D], BF16)
            xn = ring(st, "E_xn", [128, D], F32, 1)
            xnT = ring(st, "E_xnT", [128, 16, 128], F32, 1)
            xnTb = ring(st, "E_xnTb", [128, 16, 128], BF16, 1)
            rt = ring(st, "E_rt", [128, 256], F32, 2)
            gtr = ring(st, "E_gt", [128, 64], F32, 2)
            gtT = ring(st, "E_gtT", [64, 128], F32, 2)
            sel4 = sb(st, "E_sel4", [128, 4])
            self.dma("sp", sel4[:], self.sel4[:, :])
            cand = ring(st, "E_cand", [128, 1024], F32, 2)
            psr = Rot(self.ps[0:6])
            for ti in range(No // 512):
                t0 = ti * 512
                a = att.next()
                self.dma("sp", a[:], J["ATT"][:, :, t0:t0 + 512].re("h p t -> p h t"))
                self.fm_norm(a, 8, 1024, self.gatt, mixT, sq, rb, self.ps[5])
                for s in range(4):
                    r0 = t0 + s * 128
                    os_, sz = osr.next(), szr.next()
                    if J["dyn"]:
                        for jq in range(4):
                            cd = cand.next()
                            self.dma("sp", cd[:], J["OS"][jq * No + r0:jq * No + r0 + 128, :])
                            if jq == 0:
                                self.ts("dve", os_[:], cd[:], sel4[:, 0:1], None, ALU.mult)
                            else:
                                self.stt("dve", os_[:], cd[:], sel4[:, jq:jq + 1], os_[:], ALU.mult, ALU.add)
                    else:
                        self.dma("sp", os_[:], J["OS"][r0:r0 + 128, :])
                    self.dma("sp", sz[:], J["SZ"][r0:r0 + 128, :])
                    s8 = st8.next()
                    for h in range(H):
                        self.act(junk[:, 0:128], os_[:, h * 128:(h + 1) * 128], AF.Square, accum=s8[:, h:h + 1])
                    self.act(s8[:, 8:16], s8[:, 0:8], AF.Sqrt, bias=EPS, scale=1.0 / 128)
                    self.recip(s8[:, 16:24], s8[:, 8:16])
                    for h in range(H):
                        self.stt("dve", os_[:, h * 128:(h + 1) * 128], os_[:, h * 128:(h + 1) * 128],
                                 s8[:, 16 + h:17 + h], self.ggdn[:], ALU.mult, ALU.mult)
                    l = lin.next()
                    self.tt("dve", l[:], os_[:], sz[:], ALU.mult)
                    pb = self.pb[s % 2]
                    for c in range(8):
                        self.tr(pb[:, c * 128:(c + 1) * 128], l[:, c * 128:(c + 1) * 128], self.idb[:])
                    self.cp("act", mixT[:, 8:16, s * 128:(s + 1) * 128], pb[:, :].re("p (c n) -> p c n", c=8))
                for s in range(4):
                    r0 = t0 + s * 128
                    x = xo.next()
                    self.dma("sp", x[:], J["xo"][r0:r0 + 128, :])
                    y = x1.next()
                    for nb in range(4):
                        p = psr.next()
                        for m in range(16):
                            self.mm(p[:, :], mixT[:, m, s * 128:(s + 1) * 128], wout[:, m, nb * 512:(nb + 1) * 512],
                                    start=(m == 0), stop=(m == 15))
                        self.tt("dve", y[:, nb * 512:(nb + 1) * 512], p[:, :], x[:, nb * 512:(nb + 1) * 512], ALU.add)
                    self.dma("pool", J["X1"][r0:r0 + 128, :], y[:])
                    rr = rt.next()
                    self.act(junk[:], y[:], AF.Square, accum=rr[:, 0:1])
                    self.ts("dve", rr[:, 1:2], rr[:, 0:1], 1.0 / D, EPS, ALU.mult, ALU.add)
                    self.act(rr[:, 2:3], rr[:, 1:2], AF.Sqrt)
                    self.recip(rr[:, 3:4], rr[:, 2:3])
                    n2 = xn.next()
                    self.stt("dve", n2[:], y[:], rr[:, 3:4], gffn[:], ALU.mult, ALU.mult)
                    nt, ntb = xnT.next(), xnTb.next()
                    for g4 in range(4):
                        p = psr.next()
                        for c in range(4):
                            cc = g4 * 4 + c
                            self.tr(p[:, c * 128:(c + 1) * 128], n2[:, cc * 128:(cc + 1) * 128], self.idf[:])
                        self.cp("act", nt[:, g4 * 4:(g4 + 1) * 4, :], p[:, :].re("p (c n) -> p c n", c=4))
                    self.cp("pool", ntb[:], nt[:])
                    self.dma("pool", J["XN2"][:, :, r0:r0 + 128].re("c p t -> p c t"), ntb[:])
                    p = psr.next()
                    for c in range(16):
                        self.mm(p[:, 0:72], nt[:, c, :], wr[:, c, :], start=(c == 0), stop=(c == 15))
                    self.tt("dve", rr[:, 8:80], p[:, 0:72], self.br[:], ALU.add)
                    GL, EL = rr[:, 8:16], rr[:, 16:80]
                    self.rmax(rr[:, 4:5], GL)
                    self.onehot_max(rr[:, 80:88], GL, rr[:, 4:5])
                    self.ts("dve", rr[:, 5:6], rr[:, 4:5], -1.0, None, ALU.mult)
                    self.act(rr[:, 88:96], GL, AF.Exp, bias=rr[:, 5:6], scale=1.0)
                    self.rsum(rr[:, 6:7], rr[:, 88:96])
                    self.recip(rr[:, 7:8], rr[:, 6:7])
                    IG = rr[:, 96:104]
                    self.ts("dve", IG, EL[:, 0:8], rr[:, 80:81], None, ALU.mult)
                    for g in range(1, 8):
                        self.stt("dve", IG, EL[:, g * 8:(g + 1) * 8], rr[:, 80 + g:81 + g], IG, ALU.mult, ALU.add)
                    self.rmax(rr[:, 104:105], IG)
                    self.onehot_max(rr[:, 112:120], IG, rr[:, 104:105])
                    self.stt("dve", rr[:, 120:128], rr[:, 112:120], -1e30, IG, ALU.mult, ALU.add)
                    self.rmax(rr[:, 105:106], rr[:, 120:128])
                    self.onehot_max(rr[:, 128:136], rr[:, 120:128], rr[:, 105:106])
                    self.tt("dve", rr[:, 106:107], rr[:, 105:106], rr[:, 104:105], ALU.subtract)
                    self.act(rr[:, 107:108], rr[:, 106:107], AF.Exp)
                    self.ts("dve", rr[:, 108:109], rr[:, 107:108], 1.0, None, ALU.add)
                    self.recip(rr[:, 109:110], rr[:, 108:109])
                    self.tt("dve", rr[:, 110:111], rr[:, 109:110], rr[:, 7:8], ALU.mult)
                    self.tt("dve", rr[:, 111:112], rr[:, 110:111], rr[:, 107:108], ALU.mult)
                    W8 = rr[:, 136:144]
                    self.ts("dve", W8, rr[:, 112:120], rr[:, 110:111], None, ALU.mult)
                    self.stt("dve", W8, rr[:, 128:136], rr[:, 111:112], W8, ALU.mult, ALU.add)
                    gt = gtr.next()
                    for g in range(8):
                        self.ts("dve", gt[:, g * 8:(g + 1) * 8], W8, rr[:, 80 + g:81 + g], None, ALU.mult)
                    p = psr.next()
                    self.tr(p[0:64, 0:128], gt[:], self.idf[:])
                    gT = gtT.next()
                    self.cp("act", gT[:], p[0:64, 0:128])
                    self.dma("pool", J["GT"][:, r0:r0 + 128], gT[:])

    def phaseF(self, J):
        No = J["No"]
        with contextlib.ExitStack() as st:
            sb, ring = self.sb, self.ring
            gfin = sb(st, "F_g", [128, D])
            self.dma("sp", gfin[:], self.g_fin[:])
            xnT = sb(st, "F_xnT", [128, 16, 512], BF16)
            yacc = sb(st, "F_y", [128, 16, 512])
            wg = ring(st, "F_wg", [128, 16, 512], BF16, 2)
            wu = ring(st, "F_wu", [128, 16, 512], BF16, 2)
            wd = ring(st, "F_wd", [128, 4, D], BF16, 2)
            gbc = ring(st, "F_gbc", [128, 512], F32, 2)
            sg = ring(st, "F_sg", [128, 512], F32, 2)
            hu = ring(st, "F_hu", [128, 512], F32, 2)
            hT = ring(st, "F_hT", [128, 4, 512], BF16, 2)
            yt = ring(st, "F_yt", [128, D], F32, 1)
            x1 = ring(st, "F_x1", [128, D], F32, 1)
            junk = sb(st, "F_junk", [128, D], BF16)
            rr = ring(st, "F_rr", [128, 4], F32, 2)
            psr = Rot(self.ps[0:6])
            for ti in range(No // 512):
                t0 = ti * 512
                self.dma("sp", xnT[:], J["XN2"][:, :, t0:t0 + 512].re("c p t -> p c t"))
                for e in range(NEXP):
                    a, u, dn = wg.next(), wu.next(), wd.next()
                    self.dma("pool", a[:], self.w_gate[e, :, :].re("(c p) n -> p c n", p=128))
                    self.dma("pool", u[:], self.w_up[e, :, :].re("(c p) n -> p c n", p=128))
                    self.dma("pool", dn[:], self.w_down[e, :, :].re("(c p) n -> p c n", p=128))
                    gb = gbc.next()
                    GTt = J["GT"].t
                    self.dma("sp", gb[:], J["GT"][0:1, 0:512],
                             infn=lambda en, e=e, t0=t0: GTt[e:e + 1, t0:t0 + 512].partition_broadcast(128))
                    h = hT.next()
                    for j in range(4):
                        pg = psr.next()
                        for c in range(16):
                            self.mm(pg[:, :], a[:, c, j * 128:(j + 1) * 128], xnT[:, c, :], start=(c == 0), stop=(c == 15))
                        pu = psr.next()
                        for c in range(16):
                            self.mm(pu[:, :], u[:, c, j * 128:(j + 1) * 128], xnT[:, c, :], start=(c == 0), stop=(c == 15))
                        s1, h1 = sg.next(), hu.next()
                        self.act(s1[:], pg[:, :], AF.Silu)
                        self.tt("dve", h1[:], pu[:, :], gb[:], ALU.mult)
                        self.tt("pool", h[:, j, :], h1[:], s1[:], ALU.mult)
                    for dc in range(16):
                        py = psr.next()
                        for j in range(4):
                            self.mm(py[:, :], dn[:, j, dc * 128:(dc + 1) * 128], h[:, j, :], start=(j == 0), stop=(j == 3))
                        if e == 0:
                            self.cp("act" if dc % 2 else "dve", yacc[:, dc, :], py[:, :])
                        else:
                            self.tt("dve", yacc[:, dc, :], py[:, :], yacc[:, dc, :], ALU.add)
                for s in range(4):
                    r0 = t0 + s * 128
                    x = x1.next()
                    self.dma("sp", x[:], J["X1"][r0:r0 + 128, :])
                    y = yt.next()
                    for g4 in range(4):
                        p = psr.next()
                        for c in range(4):
                            cc = g4 * 4 + c
                            self.tr(p[:, c * 128:(c + 1) * 128], yacc[:, cc, s * 128:(s + 1) * 128], self.idf[:])
                        self.tt("dve", y[:, g4 * 512:(g4 + 1) * 512], p[:, :], x[:, g4 * 512:(g4 + 1) * 512], ALU.add)
                    r = rr.next()
                    self.act(junk[:], y[:], AF.Square, accum=r[:, 0:1])
                    self.ts("dve", r[:, 1:2], r[:, 0:1], 1.0 / D, EPS, ALU.mult, ALU.add)
                    self.act(r[:, 2:3], r[:, 1:2], AF.Sqrt)
                    self.recip(r[:, 3:4], r[:, 2:3])
                    self.stt("dve", y[:], y[:], r[:, 3:4], gfin[:], ALU.mult, ALU.mult)
                    self.out_ops.append(self.dma("pool", J["y"][r0:r0 + 128, :], y[:]))


class _Sub:
    def __init__(self, tile, c0):
        self.tile, self.c0 = tile, c0

    def __getitem__(self, idx):
        p, c, f = idx
        return self.tile[p, c + self.c0, f]


def _rope_tables(pos):
    inv = (np.float32(10000.0) ** (-np.arange(0, 64, 2, dtype=np.float32) / np.float32(64))).astype(np.float32)
    ang = pos.astype(np.float32)[:, None] * inv[None, :]
    cos, sin = np.cos(ang).astype(np.float32), np.sin(ang).astype(np.float32)
    cs = np.concatenate([cos, cos], axis=1).T
    sn = np.concatenate([-sin, sin], axis=1).T
    return np.ascontiguousarray(cs), np.ascontiguousarray(sn)


def _rep(v, n=128):
    return np.ascontiguousarray(np.broadcast_to(np.asarray(v, np.float32).reshape(1, -1), (n, v.size)))


def _fm(v, nch):
    return np.ascontiguousarray(np.asarray(v, np.float32).reshape(nch, 128).T)


def make_in_maps(inp, S_S, S_P, NoP, ncore=8):
    f = lambda a: np.ascontiguousarray(np.asarray(a, np.float32))
    w_in = f(inp["w_in"])[0]
    cq, ckv, kpe = w_in[:, 0:512], w_in[:, 512:1024], w_in[:, 1024:1088]
    qkv, z, a, bt = w_in[:, 1088:4160], w_in[:, 4160:5184], w_in[:, 5184:5200], w_in[:, 5200:5216]
    kpe_sw = np.concatenate([kpe[:, 32:], kpe[:, :32]], axis=1)
    w_ctx = np.concatenate([ckv, qkv, kpe, kpe_sw, a, bt, np.zeros((D, 96), np.float32)], axis=1)
    w_own = np.concatenate([cq, z], axis=1)
    wuq = f(inp["w_uq"])[0].reshape(512, 8, 192)
    w_uqn = wuq[:, :, :128].reshape(512, 1024)
    pe = wuq[:, :, 128:]
    w_uqp = pe.reshape(512, 512)
    w_uqps = np.concatenate([pe[:, :, 32:], pe[:, :, :32]], axis=2).reshape(512, 512)
    wukv = f(inp["w_ukv"])[0].reshape(512, 8, 256)
    w_uk = wukv[:, :, :128].reshape(512, 1024)
    w_uv = wukv[:, :, 128:].reshape(512, 1024)
    conv = f(inp["conv_w"])[0]
    conv_fm = np.ascontiguousarray(conv.reshape(5, 24, 128).transpose(2, 1, 0))
    cs_s, sn_s = _rope_tables(np.arange(S_S))
    cs_p, sn_p = _rope_tables(np.arange(S_P))
    common = dict(
        w_ctx=f(w_ctx), w_own=f(w_own), w_uqn=f(w_uqn), w_uqp=f(w_uqp), w_uqps=f(w_uqps), w_uk=f(w_uk), w_uv=f(w_uv),
        w_out=f(inp["w_out"])[0],
        w_r=f(np.concatenate([f(inp["w_router_group"])[0], f(inp["w_router_expert"])[0]], axis=1)),
        b_r=_rep(np.concatenate([f(inp["b_router_group"])[0], f(inp["b_router_expert"])[0]])),
        g_mix=_rep(f(inp["norm_mix"])[0]), g_ffn=_rep(f(inp["norm_ffn"])[0]), g_fin=_rep(f(inp["norm_final"])),
        g_q=_fm(f(inp["g_q_lora"])[0], 4), g_kv=_fm(f(inp["g_kv_lora"])[0], 4), g_att=_fm(f(inp["g_attn_out"])[0], 8),
        g_gdn=_rep(f(inp["g_gdn_out"])[0]), conv=conv_fm,
        alog=_rep(f(inp["a_log"])[0].reshape(-1)), dtb=_rep(f(inp["dt_bias"])[0].reshape(-1)),
        cs_s=cs_s, sn_s=sn_s, cs_p=cs_p, sn_p=sn_p,
        w_gate=f(inp["w_gate"])[0], w_up=f(inp["w_up"])[0], w_down=f(inp["w_down"])[0],
    )
    xp, xs = f(inp["x_prompt"]), f(inp["x_sample"])
    maps = []
    for c in range(ncore):
        bq, j = c // 4, c % 4
        m = dict(common)
        m["xs"] = xs[c]
        m["xp"] = xp[bq]
        m["xpo"] = np.ascontiguousarray(xp[bq, j * NoP:(j + 1) * NoP])
        m["cs_po"] = np.ascontiguousarray(cs_p[:, j * NoP:(j + 1) * NoP])
        m["sn_po"] = np.ascontiguousarray(sn_p[:, j * NoP:(j + 1) * NoP])
        m["own_off"] = np.array([[j * NoP]], np.int32)
        sel = np.zeros((128, 4), np.float32)
        sel[:, j] = 1.0
        m["sel4"] = sel
        maps.append(m)
    return maps


def run(inp, S_S, S_P, debug=False):
    NoP = S_P // 4
    prog = Prog(S_S, S_P, NoP, debug=debug)
    nc = prog.build()
    maps = make_in_maps(inp, S_S, S_P, NoP)
    if prog.nexp_in != NEXP:
        for m in maps:
            for k in ("w_gate", "w_up", "w_down"):
                m[k] = np.ascontiguousarray(m[k][:1])
    res = run_bass_kernel_spmd(nc, maps, core_ids=list(range(8)))
    ys = np.stack([np.asarray(res.results[c]["ys"], np.float32) for c in range(8)], axis=0)
    yp = np.stack([np.concatenate([np.asarray(res.results[bq * 4 + j]["yp"], np.float32) for j in range(4)], axis=0)
                   for bq in range(2)], axis=0)
    return (yp, ys), res, prog


def kernel(**inputs):
    (yp, ys), _, _ = run(inputs, 2048, 16384)
    return (yp, ys)
```
